# Optimizing a Trainium2 kernel written in Bass

```python
import math
import jax
import jax.numpy as jnp
from jax import lax
import numpy as np

D_MODEL = 2048
BATCH = 4
SEQ = 4096
DEPTH = 4

N_MIXERS = 2
N_A_LAYERS = (DEPTH + 1) // 2
N_B_LAYERS = DEPTH // 2
MLA_HEADS = 16
Q_LORA = 512
KV_LORA = 256
QK_NOPE = 128
QK_ROPE = 64
V_HEAD = 128
ROPE_THETA = 10000.0
Q_BLOCK = 128
MLA_IN_DIM = Q_LORA + KV_LORA + QK_ROPE
POOL_WINDOWS = (2, 4, 8, 16)
POOL_GROUPS = len(POOL_WINDOWS)
POOL_GROUP_DIM = D_MODEL // POOL_GROUPS
D_FF = 256 * ((8 * D_MODEL // 3 + 255) // 256)
N_EXPERTS = 8
TOP_K = 2
D_FF_EXPERT = D_FF
LN_EPS = 1e-5
RMS_EPS = 1e-6
DEEPNORM_ALPHA = (2.0 * DEPTH) ** 0.25
DEEPNORM_BETA = (8.0 * DEPTH) ** -0.25
N_MOD = 6

kernel_name = "hybrid_mla_pool_moe_encoder"

F32 = jnp.float32


def _layernorm(x, g, b):
    xf = x.astype(F32)
    mu = jnp.mean(xf, axis=-1, keepdims=True)
    xc = xf - mu
    var = jnp.mean(xc * xc, axis=-1, keepdims=True)
    return (xc * lax.rsqrt(var + LN_EPS) * g.astype(F32) + b.astype(F32)).astype(x.dtype)


def _rmsnorm(x, g):
    xf = x.astype(F32)
    return (xf * lax.rsqrt(jnp.mean(xf * xf, axis=-1, keepdims=True) + RMS_EPS) * g.astype(F32)).astype(x.dtype)


def _rope_tables(positions):
    inv_freq = ROPE_THETA ** (-jnp.arange(0, QK_ROPE, 2, dtype=F32) / QK_ROPE)
    ang = positions.astype(F32)[..., None] * inv_freq
    return jnp.cos(ang), jnp.sin(ang)


def _apply_rope(t, cos, sin):
    tf = t.astype(F32)
    t1, t2 = jnp.split(tf, 2, axis=-1)
    return jnp.concatenate([t1 * cos - t2 * sin, t2 * cos + t1 * sin], axis=-1).astype(t.dtype)


def _mla(h, cos, sin, w_in, q_norm, w_uq, kv_norm, w_ukv, w_o):
    b, s, _ = h.shape
    lat = h @ w_in
    q_lat, kv_lat, k_rope = jnp.split(lat, [Q_LORA, Q_LORA + KV_LORA], axis=-1)
    q = (_rmsnorm(q_lat, q_norm) @ w_uq).reshape(b, s, MLA_HEADS, QK_NOPE + QK_ROPE)
    q_nope = q[..., :QK_NOPE]
    q_rope = _apply_rope(q[..., QK_NOPE:], cos[:, :, None, :], sin[:, :, None, :])
    kv = (_rmsnorm(kv_lat, kv_norm) @ w_ukv).reshape(b, s, MLA_HEADS, QK_NOPE + V_HEAD)
    k_nope, v = kv[..., :QK_NOPE], kv[..., QK_NOPE:]
    k_rope = _apply_rope(k_rope, cos, sin)
    n_blocks = s // Q_BLOCK
    scale = (QK_NOPE + QK_ROPE) ** -0.5

    def to_blocks(t):
        return jnp.moveaxis(t.reshape(b, n_blocks, Q_BLOCK, *t.shape[2:]), 1, 0)

    def attend(qb):
        qn, qr = qb
        sc = (jnp.einsum('bqhd,bkhd->bhqk', qn, k_nope, preferred_element_type=F32)
              + jnp.einsum('bqhr,bkr->bhqk', qr, k_rope, preferred_element_type=F32))
        p = jax.nn.softmax(sc * scale, axis=-1)
        return jnp.einsum('bhqk,bkhd->bqhd', p.astype(v.dtype), v)

    o = lax.map(attend, (to_blocks(q_nope), to_blocks(q_rope)))
    o = jnp.moveaxis(o, 0, 1).reshape(b, s, MLA_HEADS * V_HEAD)
    return o @ w_o


def _pool_mixer(h, w_groups, ch_scale):
    b, s, d = h.shape
    hf = h.astype(F32)
    csum = jnp.concatenate([jnp.zeros((b, 1, d), F32), jnp.cumsum(hf, axis=1)], axis=1)
    t = jnp.arange(s)
    diffs = []
    for g, w in enumerate(POOL_WINDOWS):
        left = w // 2
        right = w - 1 - left
        lo = jnp.maximum(t - left, 0)
        hi = jnp.minimum(t + right, s - 1)
        cols = slice(g * POOL_GROUP_DIM, (g + 1) * POOL_GROUP_DIM)
        cg = csum[:, :, cols]
        win_sum = jnp.take(cg, hi + 1, axis=1) - jnp.take(cg, lo, axis=1)
        cnt = (hi - lo + 1).astype(F32)[None, :, None]
        diffs.append(win_sum / cnt - hf[:, :, cols])
    dg = jnp.stack(diffs, axis=2).astype(h.dtype)
    y = jnp.einsum('bsgc,gcd->bsgd', dg, w_groups).reshape(b, s, d)
    return y * ch_scale


def _swiglu(h, wg, wu, wd):
    return (jax.nn.silu(h @ wg) * (h @ wu)) @ wd


def _moe(h, router, wg, wu, wd):
    logits = jnp.einsum('bsd,de->bse', h, router, preferred_element_type=F32)
    probs = jax.nn.softmax(logits, axis=-1)
    top_p, top_i = lax.top_k(probs, TOP_K)
    top_p = top_p / jnp.sum(top_p, axis=-1, keepdims=True)
    gates = jnp.sum(jax.nn.one_hot(top_i, N_EXPERTS, dtype=F32) * top_p[..., None], axis=-2).astype(h.dtype)
    y = jnp.zeros_like(h)
    for e in range(N_EXPERTS):
        y = y + gates[..., e:e + 1] * _swiglu(h, wg[e], wu[e], wd[e])
    return y


def setup_inputs(seed: int = 0) -> dict:
    key = jax.random.key(seed)
    ks = jax.random.split(key, 24)
    nrm = jax.random.normal
    d, hq = D_MODEL, MLA_HEADS
    inp = {}
    inp['x'] = nrm(ks[0], (BATCH, SEQ, d), F32)
    inp['c'] = nrm(ks[1], (BATCH, d), F32)
    inp['positions'] = jnp.broadcast_to(jnp.arange(SEQ, dtype=jnp.int32), (BATCH, SEQ))
    inp['mod_w'] = nrm(ks[2], (DEPTH, d, N_MOD * d), F32) * (0.1 * d ** -0.5)
    inp['mod_b'] = nrm(ks[3], (DEPTH, N_MOD * d), F32) * 0.01
    inp['ln_g'] = 1.0 + 0.02 * nrm(ks[4], (DEPTH, 2, d), F32)
    inp['ln_b'] = 0.02 * nrm(ks[5], (DEPTH, 2, d), F32)
    inp['mla_w_in'] = nrm(ks[6], (N_A_LAYERS, d, MLA_IN_DIM), F32) * d ** -0.5
    inp['mla_q_norm'] = 1.0 + 0.02 * nrm(ks[7], (N_A_LAYERS, Q_LORA), F32)
    inp['mla_w_uq'] = nrm(ks[8], (N_A_LAYERS, Q_LORA, hq * (QK_NOPE + QK_ROPE)), F32) * Q_LORA ** -0.5
    inp['mla_kv_norm'] = 1.0 + 0.02 * nrm(ks[9], (N_A_LAYERS, KV_LORA), F32)
    inp['mla_w_ukv'] = nrm(ks[10], (N_A_LAYERS, KV_LORA, hq * (QK_NOPE + V_HEAD)), F32) * KV_LORA ** -0.5
    inp['mla_w_o'] = nrm(ks[11], (N_A_LAYERS, hq * V_HEAD, d), F32) * (DEEPNORM_BETA * (hq * V_HEAD) ** -0.5)
    inp['pool_w'] = nrm(ks[12], (N_B_LAYERS, POOL_GROUPS, POOL_GROUP_DIM, POOL_GROUP_DIM), F32) * (DEEPNORM_BETA * POOL_GROUP_DIM ** -0.5)
    inp['pool_scale'] = 1.0 + 0.1 * nrm(ks[13], (N_B_LAYERS, d), F32)
    inp['ffn_w_gate'] = nrm(ks[14], (N_A_LAYERS, d, D_FF), F32) * d ** -0.5
    inp['ffn_w_up'] = nrm(ks[15], (N_A_LAYERS, d, D_FF), F32) * d ** -0.5
    inp['ffn_w_down'] = nrm(ks[16], (N_A_LAYERS, D_FF, d), F32) * (DEEPNORM_BETA * D_FF ** -0.5)
    inp['moe_router'] = nrm(ks[17], (N_B_LAYERS, d, N_EXPERTS), F32) * d ** -0.5
    inp['moe_w_gate'] = nrm(ks[18], (N_B_LAYERS, N_EXPERTS, d, D_FF_EXPERT), F32) * d ** -0.5
    inp['moe_w_up'] = nrm(ks[19], (N_B_LAYERS, N_EXPERTS, d, D_FF_EXPERT), F32) * d ** -0.5
    inp['moe_w_down'] = nrm(ks[20], (N_B_LAYERS, N_EXPERTS, D_FF_EXPERT, d), F32) * (DEEPNORM_BETA * D_FF_EXPERT ** -0.5)
    return inp


def reference(x, c, positions, mod_w, mod_b, ln_g, ln_b,
              mla_w_in, mla_q_norm, mla_w_uq, mla_kv_norm, mla_w_ukv, mla_w_o,
              pool_w, pool_scale,
              ffn_w_gate, ffn_w_up, ffn_w_down,
              moe_router, moe_w_gate, moe_w_up, moe_w_down):
    b, _, d = x.shape
    cos, sin = _rope_tables(positions)
    c_act = jax.nn.silu(c)
    for i in range(DEPTH):
        j = i // N_MIXERS
        mod = (c_act @ mod_w[i] + mod_b[i]).reshape(b, N_MOD, d)
        sh1, sc1, g1, sh2, sc2, g2 = [mod[:, k, None, :] for k in range(N_MOD)]
        h = x * (1.0 + sc1) + sh1
        if i % N_MIXERS == 0:
            y = _mla(h, cos, sin, mla_w_in[j], mla_q_norm[j], mla_w_uq[j],
                     mla_kv_norm[j], mla_w_ukv[j], mla_w_o[j])
        else:
            y = _pool_mixer(h, pool_w[j], pool_scale[j])
        x = _layernorm(DEEPNORM_ALPHA * x + (1.0 + g1) * y, ln_g[i, 0], ln_b[i, 0])
        h = x * (1.0 + sc2) + sh2
        if i % 2 == 0:
            y = _swiglu(h, ffn_w_gate[j], ffn_w_up[j], ffn_w_down[j])
        else:
            y = _moe(h, moe_router[j], moe_w_gate[j], moe_w_up[j], moe_w_down[j])
        x = _layernorm(DEEPNORM_ALPHA * x + (1.0 + g2) * y, ln_g[i, 1], ln_b[i, 1])
    return x
```

```python
from contextlib import ExitStack
import math
import numpy as np
import concourse.bass as bass
import concourse.mybir as mybir
from concourse.bass_utils import run_bass_kernel_spmd

F32 = mybir.dt.float32
BF16 = mybir.dt.bfloat16
I32 = mybir.dt.int32
AF = mybir.ActivationFunctionType
ALU = mybir.AluOpType

D = 2048
SEQ = 4096
T = 2048
NT = T // 128
KD = D // 128
DFF = 5632
NFC = DFF // 128
NE = 8
ALPHA = (2.0 * 4) ** 0.25
LN_EPS = 1e-5
RMS_EPS = 1e-6
HEADS = 16
SCALE = (128 + 64) ** -0.5
TH = 1024
FG = 2


class Buf:
    __slots__ = ("name", "w", "r")

    def __init__(self, name=""):
        self.name = name
        self.w = None
        self.r = []


class K:
    def __init__(self, nc, ndma=20):
        self.nc = nc
        self.eng = {"pe": nc.tensor, "act": nc.scalar, "dve": nc.vector, "pool": nc.gpsimd, "sp": nc.sync}
        self.sem = {n: nc.alloc_semaphore("s_" + n) for n in self.eng}
        self.tick = {n: 0 for n in self.eng}
        self.waited = {n: {} for n in self.eng}
        self.dsem = {}
        self.dval = {}
        self.drr = {}
        for q in ("sp", "pool", "act"):
            self.dsem[q] = [nc.alloc_semaphore("d_%s%d" % (q, i)) for i in range(ndma)]
            self.dval[q] = [0] * ndma
            self.drr[q] = 0
        self.out_events = []

    def _semof(self, key):
        if isinstance(key, tuple):
            return self.dsem[key[0]][key[1]]
        return self.sem[key]

    def _wait(self, e, key, v):
        if self.waited[e].get(key, 0) >= v:
            return
        self.eng[e].wait_ge(self._semof(key), v)
        self.waited[e][key] = v

    def _deps(self, e, reads, writes):
        need = {}
        for b in reads:
            if b.w is not None:
                k, v = b.w
                need[k] = max(need.get(k, 0), v)
        for b in writes:
            if b.w is not None:
                k, v = b.w
                need[k] = max(need.get(k, 0), v)
            for (k, v) in b.r:
                need[k] = max(need.get(k, 0), v)
        for k, v in need.items():
            if k == e and e == "pe":
                continue
            if k == e and v > self.tick[e]:
                raise RuntimeError("self future wait")
            self._wait(e, k, v)

    def op(self, e, fn, reads=(), writes=(), sig=True):
        self._deps(e, reads, writes)
        ins = fn(self.eng[e])
        if sig:
            self.tick[e] += 1
            ins.then_inc(self.sem[e], 1)
            ev = (e, self.tick[e])
        else:
            assert e == "pe"
            ev = (e, self.tick[e] + 1)
        for b in reads:
            b.r.append(ev)
        for b in writes:
            b.w = ev
            b.r = []
        return ins

    def dma(self, q, out, in_, reads=(), writes=(), is_output=False):
        n = len(self.dsem[q])
        i = self.drr[q]
        self.drr[q] = (i + 1) % n
        key = (q, i)
        if self.dval[q][i] > 0:
            self._wait(q, key, self.dval[q][i])
        self._deps(q, reads, writes)
        self.dval[q][i] += 16
        self.eng[q].dma_start(out=out, in_=in_).then_inc(self.dsem[q][i], 16)
        ev = (key, self.dval[q][i])
        for b in reads:
            b.r.append(ev)
        for b in writes:
            b.w = ev
            b.r = []
        if is_output:
            self.out_events.append(ev)

    def barrier(self):
        for e in self.eng:
            for k in self.eng:
                if k != e and self.tick[k] > 0:
                    self._wait(e, k, self.tick[k])
            for q in self.dsem:
                for i, v in enumerate(self.dval[q]):
                    if v > 0:
                        self._wait(e, (q, i), v)

    def finish(self):
        for (k, v) in self.out_events:
            self._wait("sp", k, v)
        self.barrier()


def bufs(n, name=""):
    return [Buf(name + str(i)) for i in range(n)]


class Ctx:
    pass


_uid = [0]


def sb(k, es, name, shape, dt):
    _uid[0] += 1
    return es.enter_context(k.nc.sbuf_tensor("%s_%d" % (name, _uid[0]), shape, dt))


def setup_consts(k, es, g):
    nc = k.nc
    g.ident_f = sb(k, es, "identf", [128, 128], F32)
    g.ident = sb(k, es, "identb", [128, 128], BF16)
    g.ones = sb(k, es, "ones", [128, 128], BF16)
    g.crep = sb(k, es, "crep", [128, KD, 128], BF16)
    g.ccol = sb(k, es, "ccol", [128, KD], F32)
    g.b_const = Buf("const")
    b = g.b_const
    k.dma("sp", g.ident_f[:], g.ident_dram, writes=[b])
    k.dma("sp", g.ccol[:], g.c_dram, writes=[b])
    k.op("dve", lambda e: e.tensor_copy(g.ident[:], g.ident_f[:]), reads=[b], writes=[b])
    k.op("dve", lambda e: e.memset(g.ones[:], 1.0), writes=[b])
    k.op("act", lambda e: e.activation(out=g.ccol[:], in_=g.ccol[:], func=AF.Silu), reads=[b], writes=[b])
    for kk in range(KD):
        k.op("dve", lambda e, kk=kk: e.tensor_scalar(out=g.crep[:, kk, :], in0=g.ones[:], scalar1=g.ccol[:, kk:kk + 1],
                                                      scalar2=None, op0=ALU.mult), reads=[b], writes=[b])


def compute_mod(k, g, es, sub, psum_bank, b_ps, extra_scale=None, want=("SH", "A", "G", "LN")):
    nc = k.nc
    t = Ctx()
    t.b = Buf("mod")
    outs = {}
    for j, nm in enumerate(("SH", "A", "G")):
        if nm in want:
            outs[j] = sb(k, es, "mod" + nm, [128, D], F32)
            setattr(t, nm, outs[j])
    if "LN" in want:
        t.LG = sb(k, es, "lnG", [128, D], F32)
        t.LB = sb(k, es, "lnB", [128, D], F32)
        k.dma("sp", t.LG[:], g.ln_g_dram[sub:sub + 1, :].partition_broadcast(128), writes=[t.b])
        k.dma("sp", t.LB[:], g.ln_b_dram[sub:sub + 1, :].partition_broadcast(128), writes=[t.b])
    with ExitStack() as les:
        wst = [sb(k, les, "modw", [128, KD, 512], BF16) for i in range(2)]
        bw = bufs(2, "modw")
        bias = sb(k, les, "modbias", [128, D], F32)
        bb = Buf("modbias")
        it = 0
        for j in sorted(outs):
            col0 = (sub * 3 + j) * D
            k.dma("sp", bias[:], g.mod_b_dram[0:1, col0:col0 + D].partition_broadcast(128), writes=[bb])
            for n in range(4):
                s = it % 2
                it += 1
                c0 = col0 + n * 512
                k.dma("pool", wst[s][:], g.mod_w_dram[:, c0:c0 + 512].rearrange("(k p) f -> p k f", p=128), writes=[bw[s]])
                for kk in range(KD):
                    k.op("pe", lambda e, kk=kk, s=s: e.matmul(psum_bank, g.crep[:, kk, :], wst[s][:, kk, :],
                                                               start=(kk == 0), stop=(kk == KD - 1)),
                         reads=[g.b_const, bw[s]], writes=[b_ps], sig=(kk == KD - 1))
                osl = outs[j][:, n * 512:(n + 1) * 512]
                if j == 0:
                    k.op("dve", lambda e: e.tensor_tensor(out=osl, in0=psum_bank, in1=bias[:, n * 512:(n + 1) * 512], op=ALU.add),
                         reads=[b_ps, bb], writes=[t.b])
                else:
                    k.op("dve", lambda e: e.scalar_tensor_tensor(out=osl, in0=psum_bank, scalar=1.0, in1=bias[:, n * 512:(n + 1) * 512],
                                                                  op0=ALU.add, op1=ALU.add),
                         reads=[b_ps, bb], writes=[t.b])
        if extra_scale is not None:
            k.dma("sp", bias[:], extra_scale.partition_broadcast(128), writes=[bb])
            k.op("dve", lambda e: e.tensor_tensor(out=t.G[:], in0=t.G[:], in1=bias[:], op=ALU.mult), reads=[bb, t.b], writes=[t.b])
        k.barrier()
    return t


def prologue(k, g, mod, xt, bx, tmp, btmp, hb, bhb, valid_col=None, bvalid=None):
    k.op("pool", lambda e: e.tensor_tensor(out=tmp[:], in0=xt[:], in1=mod.A[:], op=ALU.mult), reads=[bx, mod.b], writes=[btmp])
    if valid_col is None:
        k.op("dve", lambda e: e.tensor_tensor(out=hb[:], in0=tmp[:], in1=mod.SH[:], op=ALU.add), reads=[btmp, mod.b], writes=[bhb])
    else:
        k.op("dve", lambda e: e.tensor_tensor(out=tmp[:], in0=tmp[:], in1=mod.SH[:], op=ALU.add), reads=[btmp, mod.b], writes=[btmp])
        k.op("act", lambda e: e.activation(out=hb[:], in_=tmp[:], func=AF.Identity, scale=valid_col), reads=[btmp, bvalid], writes=[bhb])


def transpose_tile(k, g, hb, bhb, pT, bpT, nblk=KD):
    for kk in range(nblk):
        k.op("pe", lambda e, kk=kk: e.transpose(out=pT[:, kk, :], in_=hb[:, kk * 128:(kk + 1) * 128], identity=g.ident[:]),
             reads=[bhb, g.b_const], writes=[bpT], sig=(kk == nblk - 1))


def epilogue(k, g, mod, ep, xt, bx, y_ap, by, out_dram):
    z, bz = ep.z, ep.bz
    k.op("dve", lambda e: e.tensor_tensor(out=z[:], in0=y_ap, in1=mod.G[:], op=ALU.mult), reads=[by, mod.b], writes=[bz])
    k.op("dve", lambda e: e.scalar_tensor_tensor(out=z[:], in0=xt[:], scalar=ALPHA, in1=z[:], op0=ALU.mult, op1=ALU.add),
         reads=[bx, bz], writes=[bz])
    for c in range(4):
        k.op("dve", lambda e, c=c: e.bn_stats(out=ep.st[:, c, :], in_=z[:, c * 512:(c + 1) * 512]), reads=[bz], writes=[ep.bst])
    k.op("dve", lambda e: e.bn_aggr(out=ep.mv[:], in_=ep.st[:]), reads=[ep.bst], writes=[ep.bmv])
    k.op("act", lambda e: e.activation(out=ep.rs[:, 0:1], in_=ep.mv[:, 1:2], func=AF.Sqrt, bias=LN_EPS, scale=1.0),
         reads=[ep.bmv], writes=[ep.brs])
    k.op("dve", lambda e: e.reciprocal(out=ep.rs[:, 0:1], in_=ep.rs[:, 0:1]), reads=[ep.brs], writes=[ep.brs])
    k.op("dve", lambda e: e.scalar_tensor_tensor(out=ep.rs[:, 1:2], in0=ep.mv[:, 0:1], scalar=-1.0, in1=ep.rs[:, 0:1], op0=ALU.mult, op1=ALU.mult),
         reads=[ep.bmv, ep.brs], writes=[ep.brs])
    k.op("act", lambda e: e.activation(out=z[:], in_=z[:], func=AF.Identity, scale=ep.rs[:, 0:1], bias=ep.rs[:, 1:2]),
         reads=[bz, ep.brs], writes=[bz])
    k.op("pool", lambda e: e.tensor_tensor(out=z[:], in0=z[:], in1=mod.LG[:], op=ALU.mult), reads=[bz, mod.b], writes=[bz])
    k.op("pool", lambda e: e.tensor_tensor(out=ep.xo[:], in0=z[:], in1=mod.LB[:], op=ALU.add), reads=[bz, mod.b], writes=[ep.bxo])
    k.dma("sp", out_dram, ep.xo[:], reads=[ep.bxo], is_output=True)


def make_ep(k, es, name):
    ep = Ctx()
    ep.z = sb(k, es, name + "_z", [128, D], F32)
    ep.xo = sb(k, es, name + "_xo", [128, D], F32)
    ep.st = sb(k, es, name + "_st", [128, 4, 6], F32)
    ep.mv = sb(k, es, name + "_mv", [128, 2], F32)
    ep.rs = sb(k, es, name + "_rs", [128, 2], F32)
    ep.bz, ep.bxo, ep.bst, ep.bmv, ep.brs = bufs(5, name)
    return ep


def bank(g, i):
    return g.pss[i // 4][:, (i % 4) * 512:(i % 4 + 1) * 512]


def setup_psum(k, es, g):
    nc = k.nc
    g.pss = [es.enter_context(nc.psum_tensor("psA", [128, 2048], F32)), es.enter_context(nc.psum_tensor("psB", [128, 2048], F32))]
    g.bps = bufs(8, "psbank")


def ffn_sublayer(k, g, x_in, x_out, wg_d, wu_d, wd_d, n_exp, router_d=None):
    nc = k.nc
    NTT = TH // 128
    with ExitStack() as es:
        mod = compute_mod(k, g, es, 1, bank(g, 7), g.bps[7])
        hT = sb(k, es, "hT", [128, KD, TH], BF16)
        b_hT = bufs(NTT, "hT")
        yacc = sb(k, es, "yacc", [128, NTT, D], F32)
        b_y = [bufs(4, "y%d_" % t) for t in range(NTT)]
        gates = sb(k, es, "gates", [128, NTT, NE], F32)
        b_g = Buf("gates")
        if router_d is not None:
            Rb = sb(k, es, "Rb", [128, KD, NE], BF16)
            b_R = Buf("Rb")
            k.dma("pool", Rb[:], router_d.rearrange("(k p) e -> p k e", p=128), writes=[b_R])
        for half in range(T // TH):
            with ExitStack() as pes:
                xt = [sb(k, pes, "xt", [128, D], F32) for _ in range(2)]
                bxt = bufs(2, "xt")
                tmp = sb(k, pes, "tmp", [128, D], F32)
                btmp = Buf("tmp")
                hb = [sb(k, pes, "hb", [128, D], BF16) for _ in range(2)]
                bhb = bufs(2, "hb")
                lg = sb(k, pes, "lg", [128, NE], F32)
                t8 = sb(k, pes, "t8", [128, NE], F32)
                ee = sb(k, pes, "ee", [128, NE], F32)
                mk = sb(k, pes, "mk", [128, NE], F32)
                sm = sb(k, pes, "sm", [128, 4], F32)
                bgt = Buf("gt")
                if router_d is None:
                    k.op("pool", lambda e: e.memset(gates[:], 1.0), writes=[b_g])
                k.op("pool", lambda e: e.memset(yacc[:], 0.0), writes=[b for bb_ in b_y for b in bb_])
                for t in range(NTT):
                    s = t % 2
                    tok0 = half * TH + t * 128
                    k.dma("sp", xt[s][:], x_in[tok0:tok0 + 128, :], writes=[bxt[s]])
                    prologue(k, g, mod, xt[s], bxt[s], tmp, btmp, hb[s], bhb[s])
                    for hh in range(2):
                        pT = bank(g, hh).bitcast(BF16)
                        for kk in range(8):
                            kc = hh * 8 + kk
                            k.op("pe", lambda e, kk=kk, kc=kc, pT=pT: e.transpose(out=pT[:, kk * 128:(kk + 1) * 128],
                                                                                   in_=hb[s][:, kc * 128:(kc + 1) * 128], identity=g.ident[:]),
                                 reads=[bhb[s], g.b_const], writes=[g.bps[hh]], sig=(kk == 7))
                        dst = hT[:, hh * 8:(hh + 1) * 8, t * 128:(t + 1) * 128]
                        src = pT.rearrange("p (k t) -> p k t", k=8)
                        if hh == 0:
                            k.op("act", lambda e: e.activation(out=dst, in_=src, func=AF.Copy), reads=[g.bps[hh]], writes=[b_hT[t]])
                        else:
                            k.op("dve", lambda e: e.tensor_copy(dst, src), reads=[g.bps[hh]], writes=[b_hT[t]])
                    if router_d is not None:
                        lps = bank(g, 6)[:, 0:NE]
                        for kk in range(KD):
                            k.op("pe", lambda e, kk=kk: e.matmul(lps, hT[:, kk, t * 128:(t + 1) * 128], Rb[:, kk, :],
                                                                  start=(kk == 0), stop=(kk == KD - 1)),
                                 reads=[b_hT[t], b_R], writes=[g.bps[6]], sig=(kk == KD - 1))
                        k.op("dve", lambda e: e.tensor_copy(lg[:], lps), reads=[g.bps[6]], writes=[bgt])
                        k.op("dve", lambda e: e.max(out=t8[:], in_=lg[:]), reads=[bgt], writes=[bgt])
                        k.op("dve", lambda e: e.tensor_scalar(out=sm[:, 0:1], in0=t8[:, 0:1], scalar1=-1.0, scalar2=None, op0=ALU.mult),
                             reads=[bgt], writes=[bgt])
                        k.op("act", lambda e: e.activation(out=ee[:], in_=lg[:], func=AF.Exp, bias=sm[:, 0:1], scale=1.0), reads=[bgt], writes=[bgt])
                        k.op("dve", lambda e: e.tensor_scalar(out=mk[:], in0=lg[:], scalar1=t8[:, 1:2], scalar2=None, op0=ALU.is_ge),
                             reads=[bgt], writes=[bgt])
                        k.op("dve", lambda e: e.tensor_tensor(out=ee[:], in0=ee[:], in1=mk[:], op=ALU.mult), reads=[bgt], writes=[bgt])
                        k.op("dve", lambda e: e.reduce_sum(out=sm[:, 1:2], in_=ee[:], axis=mybir.AxisListType.X), reads=[bgt], writes=[bgt])
                        k.op("dve", lambda e: e.reciprocal(out=sm[:, 2:3], in_=sm[:, 1:2]), reads=[bgt], writes=[bgt])
                        k.op("dve", lambda e: e.tensor_scalar(out=gates[:, t, :], in0=ee[:], scalar1=sm[:, 2:3], scalar2=None, op0=ALU.mult),
                             reads=[bgt], writes=[b_g])
                k.barrier()
            with ExitStack() as mes:
                wgb = [sb(k, mes, "wgb", [128, KD, FG * 128], BF16) for _ in range(2)]
                wub = [sb(k, mes, "wub", [128, KD, FG * 128], BF16) for _ in range(2)]
                wdb = [sb(k, mes, "wdb", [128, FG, D], BF16) for _ in range(2)]
                b_wg, b_wu, b_wd = bufs(2, "wg"), bufs(2, "wu"), bufs(2, "wd")
                actb = [sb(k, mes, "actb", [128, FG, TH], BF16) for _ in range(2)]
                b_act = [[bufs(TH // 512, "a") for _ in range(FG)] for _ in range(2)]
                sil = [sb(k, mes, "sil", [128, 512], BF16) for _ in range(2)]
                b_sil = bufs(2, "sil")
                groups = [(e_, gi) for e_ in range(n_exp) for gi in range(NFC // FG)]
                NG = len(groups)
                cnt = {"up": 0, "dn": 0}

                def load(idx):
                    e_, gi = groups[idx]
                    s = idx % 2
                    f0 = gi * FG * 128
                    k.dma("pool", wgb[s][:], wg_d[e_][:, f0:f0 + FG * 128].rearrange("(k p) f -> p k f", p=128), writes=[b_wg[s]])
                    k.dma("pool", wub[s][:], wu_d[e_][:, f0:f0 + FG * 128].rearrange("(k p) f -> p k f", p=128), writes=[b_wu[s]])
                    k.dma("pool", wdb[s][:], wd_d[e_][f0:f0 + FG * 128, :].rearrange("(c p) d -> p c d", p=128), writes=[b_wd[s]])

                def up_block(idx, q):
                    s = idx % 2
                    f, tt = q // (TH // 512), q % (TH // 512)
                    r = cnt["up"] % 2
                    cnt["up"] += 1
                    gb, ub = 2 * r, 2 * r + 1
                    for (bk, wb, bw) in ((gb, wgb, b_wg), (ub, wub, b_wu)):
                        for kk in range(KD):
                            k.op("pe", lambda e, kk=kk, bk=bk, wb=wb: e.matmul(bank(g, bk), wb[s][:, kk, f * 128:(f + 1) * 128],
                                                                                hT[:, kk, tt * 512:(tt + 1) * 512],
                                                                                start=(kk == 0), stop=(kk == KD - 1)),
                                 reads=[bw[s]] + b_hT[tt * 4:(tt + 1) * 4], writes=[g.bps[bk]], sig=(kk == KD - 1))
                    k.op("act", lambda e: e.activation(out=sil[r][:], in_=bank(g, gb), func=AF.Silu), reads=[g.bps[gb]], writes=[b_sil[r]])
                    k.op("dve", lambda e: e.tensor_tensor(out=actb[s][:, f, tt * 512:(tt + 1) * 512], in0=bank(g, ub), in1=sil[r][:], op=ALU.mult),
                         reads=[g.bps[ub], b_sil[r]], writes=[b_act[s][f][tt]])

                def down_block(idx, q):
                    e_, gi = groups[idx]
                    s = idx % 2
                    ntk = NTT // 4
                    for tk in range(q * ntk, (q + 1) * ntk):
                        for dt in range(4):
                            bk = 4 + cnt["dn"] % 4
                            cnt["dn"] += 1
                            for f in range(FG):
                                k.op("pe", lambda e, f=f, bk=bk: e.matmul(bank(g, bk), actb[s][:, f, tk * 128:(tk + 1) * 128],
                                                                           wdb[s][:, f, dt * 512:(dt + 1) * 512],
                                                                           start=(f == 0), stop=(f == FG - 1)),
                                     reads=[b_act[s][f][tk // 4], b_wd[s]], writes=[g.bps[bk]], sig=(f == FG - 1))
                            ysl = yacc[:, tk, dt * 512:(dt + 1) * 512]
                            k.op("dve", lambda e, bk=bk, ysl=ysl: e.scalar_tensor_tensor(out=ysl, in0=bank(g, bk), scalar=gates[:, tk, e_:e_ + 1],
                                                                                          in1=ysl, op0=ALU.mult, op1=ALU.add),
                                 reads=[g.bps[bk], b_g, b_y[tk][dt]], writes=[b_y[tk][dt]])

                load(0)
                if NG > 1:
                    load(1)
                for q in range(4):
                    up_block(0, q)
                for idx in range(NG):
                    for q in range(4):
                        if idx + 1 < NG:
                            up_block(idx + 1, q)
                        down_block(idx, q)
                    if idx + 2 < NG:
                        load(idx + 2)
                k.barrier()
            with ExitStack() as ees:
                ep = make_ep(k, ees, "epf")
                xe = [sb(k, ees, "xe", [128, D], F32) for _ in range(2)]
                bxe = bufs(2, "xe")
                for t in range(NTT):
                    s = t % 2
                    tok0 = half * TH + t * 128
                    k.dma("sp", xe[s][:], x_in[tok0:tok0 + 128, :], writes=[bxe[s]])
                    epilogue(k, g, mod, ep, xe[s], bxe[s], yacc[:, t, :], Buf(), x_out[tok0:tok0 + 128, :])
                k.barrier()


def pool_sublayer(k, g, x_ext, valid_d, band_d, pw_d, pscale_d, x_out):
    nc = k.nc
    with ExitStack() as es:
        mod = compute_mod(k, g, es, 0, bank(g, 7), g.bps[7], extra_scale=pscale_d)
        band = sb(k, es, "band", [128, 12, 128], BF16)
        pw = sb(k, es, "pw", [128, 16, 512], BF16)
        vcol = sb(k, es, "vcol", [128, 18], F32)
        vrep = sb(k, es, "vrep", [128, 18, 128], BF16)
        b_c = Buf("poolc")
        k.dma("pool", band[:], band_d, writes=[b_c])
        for gq in range(4):
            k.dma("pool", pw[:, gq * 4:(gq + 1) * 4, :], pw_d[gq].rearrange("(c p) d -> p c d", p=128), writes=[b_c])
        k.dma("sp", vcol[:], valid_d, writes=[b_c])
        for e_ in range(18):
            k.op("dve", lambda e, e_=e_: e.tensor_scalar(out=vrep[:, e_, :], in0=g.ones[:], scalar1=vcol[:, e_:e_ + 1], scalar2=None, op0=ALU.mult),
                 reads=[b_c, g.b_const], writes=[b_c])
        hbuf = [sb(k, es, "hbuf", [128, D], BF16) for _ in range(4)]
        b_h = bufs(4, "hbuf")
        xt = [sb(k, es, "xtp", [128, D], F32) for _ in range(2)]
        bxt = bufs(2, "xtp")
        tmp = sb(k, es, "tmpp", [128, D], F32)
        btmp = Buf("tmpp")
        ep = make_ep(k, es, "epp")
        xe = [sb(k, es, "xep", [128, D], F32) for _ in range(2)]
        bxe = bufs(2, "xep")
        dg = [sb(k, es, "dg", [128, 4, 128], BF16) for _ in range(2)]
        b_dg = bufs(2, "dg")
        inv = [sb(k, es, "inv", [128, 128], F32) for _ in range(2)]
        b_inv = bufs(2, "inv")
        tw = [sb(k, es, "tw", [128, 128], F32) for _ in range(2)]
        b_tw = bufs(2, "tw")
        y_ps = g.pss[0]
        win_ps, hT_ps, cnt_ps = bank(g, 4), bank(g, 5), bank(g, 6)

        def pro(e_):
            s = e_ % 2
            k.dma("sp", xt[s][:], x_ext[e_ * 128:(e_ + 1) * 128, :], writes=[bxt[s]])
            prologue(k, g, mod, xt[s], bxt[s], tmp, btmp, hbuf[e_ % 4], b_h[e_ % 4], valid_col=vcol[:, e_:e_ + 1], bvalid=b_c)

        pro(0)
        pro(1)
        it = 0
        for j in range(NT):
            pro(j + 2)
            k.dma("sp", xe[j % 2][:], x_ext[(j + 1) * 128:(j + 2) * 128, :], writes=[bxe[j % 2]])
            for gq in range(4):
                r = it % 2
                it += 1
                for s3 in range(3):
                    k.op("pe", lambda e, s3=s3: e.matmul(cnt_ps[:, 0:128], vrep[:, j + s3, :], band[:, gq * 3 + s3, :], start=(s3 == 0), stop=(s3 == 2)),
                         reads=[b_c], writes=[g.bps[6]], sig=(s3 == 2))
                for cb in range(4):
                    d0 = gq * 512 + cb * 128
                    for s3 in range(3):
                        k.op("pe", lambda e, s3=s3, cb=cb, d0=d0: e.matmul(win_ps[:, cb * 128:(cb + 1) * 128], hbuf[(j + s3) % 4][:, d0:d0 + 128],
                                                                             band[:, gq * 3 + s3, :], start=(s3 == 0), stop=(s3 == 2)),
                             reads=[b_h[(j + s3) % 4], b_c], writes=[g.bps[4]], sig=(s3 == 2 and cb == 3))
                    k.op("pe", lambda e, cb=cb, d0=d0: e.matmul(hT_ps[:, cb * 128:(cb + 1) * 128], hbuf[(j + 1) % 4][:, d0:d0 + 128], g.ident[:],
                                                                 start=True, stop=True),
                         reads=[b_h[(j + 1) % 4], g.b_const], writes=[g.bps[5]], sig=(cb == 3))
                k.op("dve", lambda e: e.reciprocal(out=inv[r][:], in_=cnt_ps[:, 0:128]), reads=[g.bps[6]], writes=[b_inv[r]])
                for cb in range(4):
                    k.op("dve", lambda e, cb=cb: e.tensor_tensor(out=tw[cb % 2][:], in0=win_ps[:, cb * 128:(cb + 1) * 128], in1=inv[r][:], op=ALU.mult),
                         reads=[g.bps[4], b_inv[r]], writes=[b_tw[cb % 2]])
                    k.op("dve", lambda e, cb=cb: e.tensor_tensor(out=dg[r][:, cb, :], in0=tw[cb % 2][:], in1=hT_ps[:, cb * 128:(cb + 1) * 128], op=ALU.subtract),
                         reads=[g.bps[5], b_tw[cb % 2]], writes=[b_dg[r]])
                for cb in range(4):
                    k.op("pe", lambda e, cb=cb: e.matmul(y_ps[:, gq * 512:(gq + 1) * 512], dg[r][:, cb, :], pw[:, gq * 4 + cb, :],
                                                          start=(cb == 0), stop=(cb == 3)),
                         reads=[b_dg[r], b_c], writes=[g.bps[gq]], sig=(cb == 3))
            by = Buf("ycomb")
            by.w = ("pe", k.tick["pe"])
            epilogue(k, g, mod, ep, xe[j % 2], bxe[j % 2], y_ps[:, :], by, x_out[j * 128:(j + 1) * 128, :])
            for gq in range(4):
                g.bps[gq].r.append(by.r[-1])
        k.barrier()


def common_inputs(nc, g):
    g.ident_dram = nc.dram_tensor("ident", [128, 128], F32, kind="ExternalInput").ap()
    g.c_dram = nc.dram_tensor("c_pk", [128, KD], F32, kind="ExternalInput").ap()


def layer_inputs(nc, g, sfx=""):
    g.mod_w_dram = nc.dram_tensor("mod_w" + sfx, [D, 6 * D], F32, kind="ExternalInput").ap()
    g.mod_b_dram = nc.dram_tensor("mod_b" + sfx, [1, 6 * D], F32, kind="ExternalInput").ap()
    g.ln_g_dram = nc.dram_tensor("ln_g" + sfx, [2, D], F32, kind="ExternalInput").ap()
    g.ln_b_dram = nc.dram_tensor("ln_b" + sfx, [2, D], F32, kind="ExternalInput").ap()


def build_layer_b():
    nc = bass.Bass("TRN2", target_bir_lowering=False)
    g = Ctx()
    k = K(nc)
    common_inputs(nc, g)
    layer_inputs(nc, g)
    x_ext = nc.dram_tensor("x_ext", [18 * 128, D], F32, kind="ExternalInput").ap()
    valid = nc.dram_tensor("valid", [128, 18], F32, kind="ExternalInput").ap()
    band = nc.dram_tensor("band", [128, 12, 128], F32, kind="ExternalInput").ap()
    pw = nc.dram_tensor("pool_w", [4, 512, 512], F32, kind="ExternalInput").ap()
    pscale = nc.dram_tensor("pool_scale", [1, D], F32, kind="ExternalInput").ap()
    router = nc.dram_tensor("router", [D, NE], F32, kind="ExternalInput").ap()
    wg = nc.dram_tensor("moe_wg", [NE, D, DFF], F32, kind="ExternalInput").ap()
    wu = nc.dram_tensor("moe_wu", [NE, D, DFF], F32, kind="ExternalInput").ap()
    wd = nc.dram_tensor("moe_wd", [NE, DFF, D], F32, kind="ExternalInput").ap()
    x1 = nc.dram_tensor("x1", [T, D], F32).ap()
    xo = nc.dram_tensor("x_out", [T, D], F32, kind="ExternalOutput").ap()
    with ExitStack() as es:
        setup_psum(k, es, g)
        setup_consts(k, es, g)
        pool_sublayer(k, g, x_ext, valid, band, pw, pscale, x1)
        k.barrier()
        ffn_sublayer(k, g, x1, xo, [wg[e] for e in range(NE)], [wu[e] for e in range(NE)], [wd[e] for e in range(NE)], NE, router_d=router)
        k.finish()
    return nc


def band_const():
    b = np.zeros((128, 12, 128), np.float32)
    tp = np.arange(128)[:, None]
    t = np.arange(128)[None, :]
    for gi, w in enumerate((2, 4, 8, 16)):
        left = w // 2
        right = w - 1 - left
        for s in range(3):
            src = tp + (s - 1) * 128
            b[:, gi * 3 + s, :] = ((src >= t - left) & (src <= t + right)).astype(np.float32)
    return b


def c_pack(cb):
    return np.ascontiguousarray(cb.reshape(KD, 128).T)


_progs = {}


def run_layer_b(x_cur, inp, i):
    j = i // 2
    if "b" not in _progs:
        _progs["b"] = build_layer_b()
    nc = _progs["b"]
    ident = np.eye(128, dtype=np.float32)
    band = band_const()
    in_maps = []
    zt = np.zeros((128, D), np.float32)
    for core in range(8):
        b, hf = core // 2, core % 2
        own = x_cur[b, hf * T:(hf + 1) * T]
        left = x_cur[b, T - 128:T] if hf == 1 else zt
        right = x_cur[b, T:T + 128] if hf == 0 else zt
        valid = np.ones((18, 128), np.float32)
        if hf == 0:
            valid[0] = 0
        else:
            valid[17] = 0
        in_maps.append({
            "ident": ident, "c_pk": c_pack(inp["c"][b]),
            "mod_w": inp["mod_w"][i], "mod_b": inp["mod_b"][i][None], "ln_g": inp["ln_g"][i], "ln_b": inp["ln_b"][i],
            "x_ext": np.concatenate([left, own, right], 0), "valid": np.ascontiguousarray(valid.T), "band": band,
            "pool_w": inp["pool_w"][j], "pool_scale": inp["pool_scale"][j][None],
            "router": inp["moe_router"][j], "moe_wg": inp["moe_w_gate"][j], "moe_wu": inp["moe_w_up"][j], "moe_wd": inp["moe_w_down"][j],
        })
    res = run_bass_kernel_spmd(nc, in_maps, core_ids=list(range(8)))
    out = np.empty_like(x_cur)
    for core in range(8):
        b, hf = core // 2, core % 2
        out[b, hf * T:(hf + 1) * T] = res.results[core]["x_out"]
    return out


def mla_sublayer(k, g, x_full, pos_d, ropec_d, w_in_d, w_in_sw_d, qn_d, kvn_d, w_uq_d, w_uq_sw_d, w_ukv_d, w_o_d, oT_dram, x_out):
    nc = k.nc
    NTF = SEQ // 128
    PI = math.pi
    ring = {"s": 0, "p": 0, "o": 0}

    def next_s():
        i = ring["s"] % 4
        ring["s"] += 1
        return i

    with ExitStack() as ab:
        kvnT = sb(k, ab, "kvnT", [128, 2, SEQ], BF16)
        b_kv = bufs(NTF, "kv")
        kropeT = sb(k, ab, "kropeT", [64, SEQ], BF16)
        b_kr = bufs(NTF, "kr")
        qlnT = sb(k, ab, "qlnT", [128, 4, T], BF16)
        b_ql = bufs(NT, "ql")
        cosT = [sb(k, ab, "cos0", [64, T], F32)]
        sinT = [sb(k, ab, "sin0", [64, T], F32)]
        b_tab = [Buf("tab0"), Buf("tab1")]
        with ExitStack() as pa:
            mod = compute_mod(k, g, pa, 0, bank(g, 7), g.bps[7], want=("SH", "A"))
            cosT.append(sb(k, pa, "cos1", [64, T], F32))
            sinT.append(sb(k, pa, "sin1", [64, T], F32))
            ropec = sb(k, pa, "ropec", [64, 2], F32)
            b_rc = Buf("ropec")
            k.dma("sp", ropec[:], ropec_d, writes=[b_rc])
            with ExitStack() as te:
                posi = sb(k, te, "posi", [64, T], I32)
                posf = sb(k, te, "posf", [64, T], F32)
                u = sb(k, te, "u", [64, T], F32)
                ki = sb(k, te, "ki", [64, T], I32)
                bp, bu = Buf("pos"), Buf("u")

                def sin_table(dst, bd, shift):
                    k.op("dve", lambda e: e.tensor_scalar(out=u[:], in0=posf[:], scalar1=1.0 / (2 * PI), scalar2=0.5 + shift / (2 * PI),
                                                          op0=ALU.mult, op1=ALU.add), reads=[bp], writes=[bu])
                    k.op("dve", lambda e: e.tensor_copy(ki[:], u[:]), reads=[bu], writes=[bu])
                    k.op("dve", lambda e: e.tensor_copy(u[:], ki[:]), reads=[bu], writes=[bu])
                    k.op("dve", lambda e: e.scalar_tensor_tensor(out=dst[:], in0=u[:], scalar=-2 * PI, in1=posf[:], op0=ALU.mult, op1=ALU.add),
                         reads=[bu, bp], writes=[bd])
                    k.op("dve", lambda e: e.tensor_scalar(out=u[:], in0=dst[:], scalar1=-PI - shift, scalar2=2 * PI, op0=ALU.is_lt, op1=ALU.mult),
                         reads=[bd], writes=[bu])
                    k.op("dve", lambda e: e.tensor_tensor(out=dst[:], in0=dst[:], in1=u[:], op=ALU.add), reads=[bd, bu], writes=[bd])
                    k.op("act", lambda e: e.activation(out=dst[:], in_=dst[:], func=AF.Sin, bias=shift, scale=1.0), reads=[bd], writes=[bd])

                for hf in range(2):
                    k.dma("sp", posi[:], pos_d[0:1, hf * T:(hf + 1) * T].partition_broadcast(64), writes=[bp])
                    k.op("dve", lambda e: e.tensor_copy(posf[:], posi[:]), reads=[bp], writes=[bp])
                    k.op("dve", lambda e: e.tensor_scalar(out=posf[:], in0=posf[:], scalar1=ropec[:, 0:1], scalar2=None, op0=ALU.mult),
                         reads=[bp, b_rc], writes=[bp])
                    bs_, bc_ = Buf("sin"), Buf("cos")
                    sin_table(sinT[hf], bs_, 0.0)
                    k.op("dve", lambda e: e.tensor_scalar(out=sinT[hf][:], in0=sinT[hf][:], scalar1=ropec[:, 1:2], scalar2=None, op0=ALU.mult),
                         reads=[bs_, b_rc], writes=[bs_])
                    sin_table(cosT[hf], bc_, PI / 2)
                k.barrier()
            Win = sb(k, pa, "Win", [128, KD, 832], BF16)
            Wsw = sb(k, pa, "Wsw", [128, KD, 64], BF16)
            qnB = sb(k, pa, "qnB", [128, 512], F32)
            kvnB = sb(k, pa, "kvnB", [128, 256], F32)
            b_w = Buf("Win")
            k.dma("pool", Win[:], w_in_d.rearrange("(k p) f -> p k f", p=128), writes=[b_w])
            k.dma("pool", Wsw[:], w_in_sw_d.rearrange("(k p) f -> p k f", p=128), writes=[b_w])
            k.dma("sp", qnB[:], qn_d.partition_broadcast(128), writes=[b_w])
            k.dma("sp", kvnB[:], kvn_d.partition_broadcast(128), writes=[b_w])
            xt = [sb(k, pa, "xta", [128, D], F32) for _ in range(2)]
            bxt = bufs(2, "xta")
            tmp = sb(k, pa, "tmpa", [128, D], F32)
            btmp = Buf("tmpa")
            hb = [sb(k, pa, "hba", [128, D], BF16) for _ in range(2)]
            bhb = bufs(2, "hba")
            hTt = [sb(k, pa, "hTt", [128, KD, 128], BF16) for _ in range(2)]
            b_hTt = bufs(2, "hTt")
            latf = sb(k, pa, "latf", [128, 512], F32)
            latb = sb(k, pa, "latb", [128, 512], BF16)
            lkf = sb(k, pa, "lkf", [128, 256], F32)
            lkb = sb(k, pa, "lkb", [128, 256], BF16)
            st = sb(k, pa, "sta", [128, 2, 6], F32)
            mv = sb(k, pa, "mva", [128, 2, 2], F32)
            ms = sb(k, pa, "msa", [128, 2], F32)
            r1 = sb(k, pa, "r1a", [64, 128], F32)
            r2 = sb(k, pa, "r2a", [64, 128], F32)
            b_lq, b_lk, b_st, b_r = Buf("lq"), Buf("lk"), [Buf("st0"), Buf("st1")], Buf("r")

            def rms(idx, src_ps, n, bsrc, normB, outf, outb, bo):
                k.op("dve", lambda e: e.bn_stats(out=st[:, idx, :], in_=src_ps), reads=[bsrc], writes=[b_st[idx]])
                k.op("dve", lambda e: e.bn_aggr(out=mv[:, idx, :], in_=st[:, idx, :]), reads=[b_st[idx]], writes=[b_st[idx]])
                k.op("dve", lambda e: e.scalar_tensor_tensor(out=ms[:, idx:idx + 1], in0=mv[:, idx, 0:1], scalar=mv[:, idx, 0:1], in1=mv[:, idx, 1:2],
                                                              op0=ALU.mult, op1=ALU.add), reads=[b_st[idx]], writes=[b_st[idx]])
                k.op("act", lambda e: e.activation(out=ms[:, idx:idx + 1], in_=ms[:, idx:idx + 1], func=AF.Sqrt, bias=RMS_EPS, scale=1.0),
                     reads=[b_st[idx]], writes=[b_st[idx]])
                k.op("dve", lambda e: e.reciprocal(out=ms[:, idx:idx + 1], in_=ms[:, idx:idx + 1]), reads=[b_st[idx]], writes=[b_st[idx]])
                k.op("act", lambda e: e.activation(out=outf, in_=src_ps, func=AF.Identity, scale=ms[:, idx:idx + 1]), reads=[bsrc, b_st[idx]], writes=[bo])
                k.op("pool", lambda e: e.tensor_tensor(out=outb, in0=outf, in1=normB, op=ALU.mult), reads=[bo, b_w], writes=[bo])

            for t in range(NTF):
                s = t % 2
                hf, tl = t // NT, (t % NT) * 128
                k.dma("sp", xt[s][:], x_full[t * 128:(t + 1) * 128, :], writes=[bxt[s]])
                prologue(k, g, mod, xt[s], bxt[s], tmp, btmp, hb[s], bhb[s])
                for hh in range(2):
                    pT = bank(g, hh).bitcast(BF16)
                    for kk in range(8):
                        kc = hh * 8 + kk
                        k.op("pe", lambda e, kk=kk, kc=kc, pT=pT: e.transpose(out=pT[:, kk * 128:(kk + 1) * 128], in_=hb[s][:, kc * 128:(kc + 1) * 128],
                                                                               identity=g.ident[:]),
                             reads=[bhb[s], g.b_const], writes=[g.bps[hh]], sig=(kk == 7))
                    dst = hTt[s][:, hh * 8:(hh + 1) * 8, :]
                    src = pT.rearrange("p (k t) -> p k t", k=8)
                    if hh == 0:
                        k.op("act", lambda e: e.activation(out=dst, in_=src, func=AF.Copy), reads=[g.bps[hh]], writes=[b_hTt[s]])
                    else:
                        k.op("dve", lambda e: e.tensor_copy(dst, src), reads=[g.bps[hh]], writes=[b_hTt[s]])
                kvps = bank(g, 2)[:, 0:256]
                for kk in range(KD):
                    k.op("pe", lambda e, kk=kk: e.matmul(kvps, hTt[s][:, kk, :], Win[:, kk, 512:768], start=(kk == 0), stop=(kk == KD - 1)),
                         reads=[b_hTt[s], b_w], writes=[g.bps[2]], sig=(kk == KD - 1))
                kra = bank(g, 3)[0:64, 0:128]
                krb = bank(g, 3)[0:64, 128:256]
                for kk in range(KD):
                    k.op("pe", lambda e, kk=kk: e.matmul(kra, Win[:, kk, 768:832], hTt[s][:, kk, :], start=(kk == 0), stop=(kk == KD - 1)),
                         reads=[b_hTt[s], b_w], writes=[g.bps[3]], sig=False)
                for kk in range(KD):
                    k.op("pe", lambda e, kk=kk: e.matmul(krb, Wsw[:, kk, :], hTt[s][:, kk, :], start=(kk == 0), stop=(kk == KD - 1)),
                         reads=[b_hTt[s], b_w], writes=[g.bps[3]], sig=(kk == KD - 1))
                if t < NT:
                    qps = bank(g, 4)
                    for kk in range(KD):
                        k.op("pe", lambda e, kk=kk: e.matmul(qps, hTt[s][:, kk, :], Win[:, kk, 0:512], start=(kk == 0), stop=(kk == KD - 1)),
                             reads=[b_hTt[s], b_w], writes=[g.bps[4]], sig=(kk == KD - 1))
                rms(0, kvps, 256, g.bps[2], kvnB[:], lkf[:], lkb[:], b_lk)
                p5 = bank(g, 5).bitcast(BF16)
                for c in range(2):
                    k.op("pe", lambda e, c=c: e.transpose(out=p5[:, c * 128:(c + 1) * 128], in_=lkb[:, c * 128:(c + 1) * 128], identity=g.ident[:]),
                         reads=[b_lk, g.b_const], writes=[g.bps[5]], sig=(c == 1))
                k.op("dve", lambda e: e.tensor_copy(kvnT[:, :, t * 128:(t + 1) * 128], p5[:, 0:256].rearrange("p (c t) -> p c t", c=2)),
                     reads=[g.bps[5]], writes=[b_kv[t]])
                k.op("dve", lambda e: e.tensor_tensor(out=r1[:], in0=kra, in1=cosT[hf][:, tl:tl + 128], op=ALU.mult), reads=[g.bps[3], b_tab[hf]], writes=[b_r])
                k.op("dve", lambda e: e.tensor_tensor(out=r2[:], in0=krb, in1=sinT[hf][:, tl:tl + 128], op=ALU.mult), reads=[g.bps[3], b_tab[hf]], writes=[b_r])
                k.op("pool", lambda e: e.tensor_tensor(out=kropeT[:, t * 128:(t + 1) * 128], in0=r1[:], in1=r2[:], op=ALU.add), reads=[b_r], writes=[b_kr[t]])
                if t < NT:
                    rms(1, qps, 512, g.bps[4], qnB[:], latf[:], latb[:], b_lq)
                    p6 = bank(g, 6).bitcast(BF16)
                    for c in range(4):
                        k.op("pe", lambda e, c=c: e.transpose(out=p6[:, c * 128:(c + 1) * 128], in_=latb[:, c * 128:(c + 1) * 128], identity=g.ident[:]),
                             reads=[b_lq, g.b_const], writes=[g.bps[6]], sig=(c == 3))
                    k.op("dve", lambda e: e.tensor_copy(qlnT[:, :, t * 128:(t + 1) * 128], p6[:, 0:512].rearrange("p (c t) -> p c t", c=4)),
                         reads=[g.bps[6]], writes=[b_ql[t]])
            k.barrier()
        with ExitStack() as pb:
            wq = [sb(k, pb, "wq", [128, 4, 192], BF16) for _ in range(2)]
            wqs = [sb(k, pb, "wqs", [128, 4, 64], BF16) for _ in range(2)]
            wkv = [sb(k, pb, "wkv", [128, 2, 256], BF16) for _ in range(2)]
            b_hw = bufs(2, "hw")
            kT = [sb(k, pb, "kT", [128, SEQ], BF16) for _ in range(2)]
            vh = [sb(k, pb, "vh", [128, NTF, 128], BF16) for _ in range(2)]
            qT = [sb(k, pb, "qT", [128, T], BF16) for _ in range(2)]
            qrT = [sb(k, pb, "qrT", [64, T], BF16) for _ in range(2)]
            b_kT, b_v, b_qT, b_qr = bufs(2, "kT"), bufs(2, "v"), bufs(2, "qT"), bufs(2, "qr")
            pTb = [sb(k, pb, "pT", [128, 512], BF16) for _ in range(4)]
            b_pT = bufs(4, "pT")
            r1 = sb(k, pb, "r1b", [64, 512], F32)
            r2 = sb(k, pb, "r2b", [64, 512], F32)
            b_r = Buf("rb")
            rec = sb(k, pb, "rec", [128, 512], F32)
            b_rec = Buf("rec")
            oTh = [sb(k, pb, "oTh", [128, T], BF16) for _ in range(2)]
            b_oTh = bufs(2, "oTh")
            b_oTd = bufs(HEADS, "oTd")
            all_kv, all_kr, all_ql = b_kv, b_kr, b_ql

            def load_w(h):
                s = h % 2
                k.dma("pool", wq[s][:], w_uq_d[:, h * 192:(h + 1) * 192].rearrange("(k p) f -> p k f", p=128), writes=[b_hw[s]])
                k.dma("pool", wqs[s][:], w_uq_sw_d[:, h * 64:(h + 1) * 64].rearrange("(k p) f -> p k f", p=128), writes=[b_hw[s]])
                k.dma("pool", wkv[s][:], w_ukv_d[:, h * 256:(h + 1) * 256].rearrange("(k p) f -> p k f", p=128), writes=[b_hw[s]])

            def proj(h):
                s = h % 2
                for tt in range(SEQ // 512):
                    bk = next_s()
                    for c in range(2):
                        k.op("pe", lambda e, c=c: e.matmul(bank(g, bk), wkv[s][:, c, 0:128], kvnT[:, c, tt * 512:(tt + 1) * 512], start=(c == 0), stop=(c == 1)),
                             reads=[b_hw[s]] + all_kv[tt * 4:(tt + 1) * 4], writes=[g.bps[bk]], sig=(c == 1))
                    k.op("dve", lambda e: e.tensor_copy(kT[s][:, tt * 512:(tt + 1) * 512], bank(g, bk)), reads=[g.bps[bk]], writes=[b_kT[s]])
                for kq in range(NTF // 4):
                    bk = next_s()
                    for j in range(4):
                        kt = kq * 4 + j
                        for c in range(2):
                            k.op("pe", lambda e, c=c, j=j, kt=kt: e.matmul(bank(g, bk)[:, j * 128:(j + 1) * 128], kvnT[:, c, kt * 128:(kt + 1) * 128], wkv[s][:, c, 128:256],
                                                                            start=(c == 0), stop=(c == 1)),
                                 reads=[b_hw[s], all_kv[kt]], writes=[g.bps[bk]], sig=(c == 1 and j == 3))
                    k.op("dve", lambda e: e.tensor_copy(vh[s][:, kq * 4:(kq + 1) * 4, :], bank(g, bk).rearrange("p (j d) -> p j d", j=4)),
                         reads=[g.bps[bk]], writes=[b_v[s]])
                for qt in range(T // 512):
                    bk = next_s()
                    for c in range(4):
                        k.op("pe", lambda e, c=c: e.matmul(bank(g, bk), wq[s][:, c, 0:128], qlnT[:, c, qt * 512:(qt + 1) * 512], start=(c == 0), stop=(c == 3)),
                             reads=[b_hw[s]] + all_ql[qt * 4:(qt + 1) * 4], writes=[g.bps[bk]], sig=(c == 3))
                    k.op("dve", lambda e: e.tensor_copy(qT[s][:, qt * 512:(qt + 1) * 512], bank(g, bk)), reads=[g.bps[bk]], writes=[b_qT[s]])
                for qt in range(T // 512):
                    ba, bb_ = next_s(), next_s()
                    for c in range(4):
                        k.op("pe", lambda e, c=c: e.matmul(bank(g, ba)[0:64, :], wq[s][:, c, 128:192], qlnT[:, c, qt * 512:(qt + 1) * 512], start=(c == 0), stop=(c == 3)),
                             reads=[b_hw[s]] + all_ql[qt * 4:(qt + 1) * 4], writes=[g.bps[ba]], sig=(c == 3))
                    for c in range(4):
                        k.op("pe", lambda e, c=c: e.matmul(bank(g, bb_)[0:64, :], wqs[s][:, c, :], qlnT[:, c, qt * 512:(qt + 1) * 512], start=(c == 0), stop=(c == 3)),
                             reads=[b_hw[s]] + all_ql[qt * 4:(qt + 1) * 4], writes=[g.bps[bb_]], sig=(c == 3))
                    k.op("dve", lambda e: e.tensor_tensor(out=r1[:], in0=bank(g, ba)[0:64, :], in1=cosT[0][:, qt * 512:(qt + 1) * 512], op=ALU.mult),
                         reads=[g.bps[ba]], writes=[b_r])
                    k.op("dve", lambda e: e.tensor_tensor(out=r2[:], in0=bank(g, bb_)[0:64, :], in1=sinT[0][:, qt * 512:(qt + 1) * 512], op=ALU.mult),
                         reads=[g.bps[bb_]], writes=[b_r])
                    k.op("pool", lambda e: e.tensor_tensor(out=qrT[s][:, qt * 512:(qt + 1) * 512], in0=r1[:], in1=r2[:], op=ALU.add), reads=[b_r], writes=[b_qr[s]])

            def attend(h):
                s = h % 2
                NKC = SEQ // 128
                for qt in range(T // 512):
                    o_ = ring["o"] % 2
                    ring["o"] += 1
                    ob, db = 4 + o_, 6 + o_
                    slots = {}

                    def qk(kc):
                        bk = next_s()
                        r = ring["p"] % 4
                        ring["p"] += 1
                        slots[kc] = r
                        k.op("pe", lambda e: e.matmul(bank(g, bk), kT[s][:, kc * 128:(kc + 1) * 128], qT[s][:, qt * 512:(qt + 1) * 512], start=True, stop=False),
                             reads=[b_kT[s], b_qT[s]], writes=[g.bps[bk]], sig=False)
                        k.op("pe", lambda e: e.matmul(bank(g, bk), kropeT[:, kc * 128:(kc + 1) * 128], qrT[s][:, qt * 512:(qt + 1) * 512], start=False, stop=True),
                             reads=[all_kr[kc], b_qr[s]], writes=[g.bps[bk]], sig=True)
                        k.op("act", lambda e: e.activation(out=pTb[r][:], in_=bank(g, bk), func=AF.Exp, scale=SCALE), reads=[g.bps[bk]], writes=[b_pT[r]])

                    def pv(kc):
                        r = slots[kc]
                        k.op("pe", lambda e: e.matmul(bank(g, ob), vh[s][:, kc, :], pTb[r][:], start=(kc == 0), stop=(kc == NKC - 1)),
                             reads=[b_v[s], b_pT[r]], writes=[g.bps[ob]], sig=False)
                        k.op("pe", lambda e: e.matmul(bank(g, db), g.ones[:], pTb[r][:], start=(kc == 0), stop=(kc == NKC - 1)),
                             reads=[g.b_const, b_pT[r]], writes=[g.bps[db]], sig=True)

                    qk(0)
                    qk(1)
                    for kc in range(NKC):
                        if kc + 2 < NKC:
                            qk(kc + 2)
                        pv(kc)
                    k.op("dve", lambda e: e.reciprocal(out=rec[:], in_=bank(g, db)), reads=[g.bps[db]], writes=[b_rec])
                    k.op("dve", lambda e: e.tensor_tensor(out=oTh[s][:, qt * 512:(qt + 1) * 512], in0=bank(g, ob), in1=rec[:], op=ALU.mult),
                         reads=[g.bps[ob], b_rec], writes=[b_oTh[s]])
                k.dma("sp", oT_dram[h], oTh[s][:], reads=[b_oTh[s]], writes=[b_oTd[h]])

            load_w(0)
            load_w(1)
            proj(0)
            for h in range(HEADS):
                if h + 1 < HEADS:
                    proj(h + 1)
                attend(h)
                if h + 2 < HEADS:
                    load_w(h + 2)
            k.barrier()
    with ExitStack() as pc:
        mod = compute_mod(k, g, pc, 0, bank(g, 7), g.bps[7], want=("G", "LN"))
        oTa = sb(k, pc, "oTa", [128, HEADS, T], BF16)
        Wo = sb(k, pc, "Wo", [128, HEADS, D], BF16)
        b_o, b_wo = Buf("oTa"), Buf("Wo")
        k.dma("sp", oTa[:], oT_dram.rearrange("h p t -> p h t"), reads=b_oTd, writes=[b_o])
        for hq in range(4):
            k.dma("pool", Wo[:, hq * 4:(hq + 1) * 4, :], w_o_d[hq * 512:(hq + 1) * 512, :].rearrange("(h p) d -> p h d", p=128), writes=[b_wo])
        ep = make_ep(k, pc, "epa")
        xe = [sb(k, pc, "xea", [128, D], F32) for _ in range(2)]
        bxe = bufs(2, "xea")
        for tk in range(NT):
            y_ps = g.pss[tk % 2]
            for dt in range(4):
                bk = (tk % 2) * 4 + dt
                for h in range(HEADS):
                    k.op("pe", lambda e, h=h: e.matmul(y_ps[:, dt * 512:(dt + 1) * 512], oTa[:, h, tk * 128:(tk + 1) * 128], Wo[:, h, dt * 512:(dt + 1) * 512],
                                                        start=(h == 0), stop=(h == HEADS - 1)),
                         reads=[b_o, b_wo], writes=[g.bps[bk]], sig=(h == HEADS - 1))
            k.dma("sp", xe[tk % 2][:], x_full[tk * 128:(tk + 1) * 128, :], writes=[bxe[tk % 2]])
            by = Buf("ycomb")
            by.w = ("pe", k.tick["pe"])
            epilogue(k, g, mod, ep, xe[tk % 2], bxe[tk % 2], y_ps[:, :], by, x_out[tk * 128:(tk + 1) * 128, :])
            for dt in range(4):
                g.bps[(tk % 2) * 4 + dt].r.append(by.r[-1])
        k.barrier()


def build_layer_a():
    nc = bass.Bass("TRN2", target_bir_lowering=False)
    g = Ctx()
    k = K(nc)
    common_inputs(nc, g)
    layer_inputs(nc, g)
    di = lambda name, shape, dt=F32: nc.dram_tensor(name, shape, dt, kind="ExternalInput").ap()
    x_full = di("x_full", [SEQ, D])
    pos = di("pos", [1, SEQ], I32)
    ropec = di("ropec", [64, 2])
    w_in = di("w_in", [D, 832])
    w_in_sw = di("w_in_sw", [D, 64])
    qn = di("qn", [1, 512])
    kvn = di("kvn", [1, 256])
    w_uq = di("w_uq", [512, 3072])
    w_uq_sw = di("w_uq_sw", [512, 1024])
    w_ukv = di("w_ukv", [256, 4096])
    w_o = di("w_o", [D, D])
    wg = di("ffn_wg", [D, DFF])
    wu = di("ffn_wu", [D, DFF])
    wd = di("ffn_wd", [DFF, D])
    oT = nc.dram_tensor("oT_scr", [HEADS, 128, T], BF16).ap()
    x1 = nc.dram_tensor("x1", [T, D], F32).ap()
    xo = nc.dram_tensor("x_out", [T, D], F32, kind="ExternalOutput").ap()
    with ExitStack() as es:
        setup_psum(k, es, g)
        setup_consts(k, es, g)
        mla_sublayer(k, g, x_full, pos, ropec, w_in, w_in_sw, qn, kvn, w_uq, w_uq_sw, w_ukv, w_o, oT, x1)
        k.barrier()
        ffn_sublayer(k, g, x1, xo, [wg], [wu], [wd], 1, router_d=None)
        k.finish()
    return nc


def rope_const():
    invf = (10000.0 ** (-(np.arange(0, 64, 2, dtype=np.float32)) / 64.0)).astype(np.float32)
    return np.stack([np.concatenate([invf, invf]), np.concatenate([-np.ones(32, np.float32), np.ones(32, np.float32)])], 1).astype(np.float32)


def swap_halves(w):
    return np.concatenate([w[..., 32:64], w[..., 0:32]], axis=-1)


def run_layer_a(x_cur, inp, i):
    j = i // 2
    if "a" not in _progs:
        _progs["a"] = build_layer_a()
    nc = _progs["a"]
    ident = np.eye(128, dtype=np.float32)
    w_in = inp["mla_w_in"][j]
    w_in_sw = np.ascontiguousarray(swap_halves(w_in[:, 768:832]))
    w_uq = inp["mla_w_uq"][j]
    w_uq_sw = np.ascontiguousarray(swap_halves(w_uq.reshape(512, HEADS, 192)[:, :, 128:192]).reshape(512, HEADS * 64))
    in_maps = []
    for core in range(8):
        b, hf = core // 2, core % 2
        own = slice(hf * T, (hf + 1) * T)
        oth = slice((1 - hf) * T, (2 - hf) * T)
        in_maps.append({
            "ident": ident, "c_pk": c_pack(inp["c"][b]),
            "mod_w": inp["mod_w"][i], "mod_b": inp["mod_b"][i][None], "ln_g": inp["ln_g"][i], "ln_b": inp["ln_b"][i],
            "x_full": np.concatenate([x_cur[b, own], x_cur[b, oth]], 0),
            "pos": np.concatenate([inp["positions"][b, own], inp["positions"][b, oth]])[None].astype(np.int32),
            "ropec": rope_const(),
            "w_in": w_in, "w_in_sw": w_in_sw, "qn": inp["mla_q_norm"][j][None], "kvn": inp["mla_kv_norm"][j][None],
            "w_uq": w_uq, "w_uq_sw": w_uq_sw, "w_ukv": inp["mla_w_ukv"][j], "w_o": inp["mla_w_o"][j],
            "ffn_wg": inp["ffn_w_gate"][j], "ffn_wu": inp["ffn_w_up"][j], "ffn_wd": inp["ffn_w_down"][j],
        })
    res = run_bass_kernel_spmd(nc, in_maps, core_ids=list(range(8)))
    out = np.empty_like(x_cur)
    for core in range(8):
        b, hf = core // 2, core % 2
        out[b, hf * T:(hf + 1) * T] = res.results[core]["x_out"]
    return out


def kernel(**inp):
    inp = {k_: np.asarray(v) for k_, v in inp.items()}
    x = np.ascontiguousarray(inp["x"], dtype=np.float32)
    for i in range(4):
        if i % 2 == 0:
            x = run_layer_a(x, inp, i)
        else:
            x = run_layer_b(x, inp, i)
    return x
```
